# Optimizing a Trainium2 kernel written in Bass

```python
import math
import jax
import jax.numpy as jnp
from jax import lax
import numpy as np

D_MODEL = 4096
BATCH = 4
SEQ = 2048
DEPTH = 1

CHUNK = 64
LEFT_CHUNKS = 8
BAND = (LEFT_CHUNKS + 1) * CHUNK
MEM_LEN = 256

A_HEADS = 16
A_HEAD_DIM = 128
MAX_REL = 128
B_HEADS = 4
B_HEAD_DIM = 128
C_HEADS = 4
C_HEAD_DIM = 256
A_WIDTH = A_HEADS * A_HEAD_DIM
B_QK_WIDTH = B_HEADS * 2 * B_HEAD_DIM
B_V_WIDTH = B_HEADS * 2 * B_HEAD_DIM
C_WIDTH = C_HEADS * C_HEAD_DIM
IN_WIDTH = 3 * A_WIDTH + 2 * B_QK_WIDTH + B_V_WIDTH + C_WIDTH
N_BRANCHES = 3
ROPE_THETA = 10000.0
Q_BLOCK = 128

N_EXPERTS = 32
TOP_K = 4
D_EXPERT = 1536
SWIGLU_LIMIT = 7.0
SWIGLU_ALPHA = 1.702
EXPERT_BLOCK = 256

LN_EPS = 1e-5
RMS_EPS = 1e-5
DEEPNORM_ALPHA = (2 * DEPTH) ** 0.25
DEEPNORM_BETA = (8 * DEPTH) ** -0.25

kernel_name = 'hybrid_chunk_stream_block'


def layer_norm(x, g, b):
    xf = x.astype(jnp.float32)
    mu = jnp.mean(xf, -1, keepdims=True)
    var = jnp.mean(jnp.square(xf - mu), -1, keepdims=True)
    return ((xf - mu) * lax.rsqrt(var + LN_EPS)).astype(x.dtype) * g + b


def rms_norm(x, g):
    xf = x.astype(jnp.float32)
    y = xf * lax.rsqrt(jnp.mean(xf * xf, -1, keepdims=True) + RMS_EPS)
    return y.astype(x.dtype) * g


def rope_tables(seq, dim, dtype):
    inv = 1.0 / (ROPE_THETA ** (jnp.arange(0, dim, 2, dtype=jnp.float32) / dim))
    ang = jnp.arange(seq, dtype=jnp.float32)[:, None] * inv[None, :]
    ang = jnp.concatenate([ang, ang], -1)
    return jnp.cos(ang).astype(dtype), jnp.sin(ang).astype(dtype)


def apply_rope(x, cos, sin):
    x1, x2 = jnp.split(x, 2, axis=-1)
    rot = jnp.concatenate([-x2, x1], -1)
    shape = (1, cos.shape[0]) + (1,) * (x.ndim - 3) + (cos.shape[1],)
    return x * cos.reshape(shape) + rot * sin.reshape(shape)


def chunk_relbias_attention(q, k, v, rel_bias):
    b, s, h, dh = q.shape
    nc = s // CHUNK
    pad = LEFT_CHUNKS * CHUNK
    kp = jnp.pad(k, ((0, 0), (pad, 0), (0, 0), (0, 0)))
    vp = jnp.pad(v, ((0, 0), (pad, 0), (0, 0), (0, 0)))
    qc = jnp.moveaxis(q.reshape(b, nc, CHUNK, h, dh), 1, 0)
    rel = jnp.arange(CHUNK)[:, None] - jnp.arange(BAND)[None, :] + pad
    bias = rel_bias[:, jnp.clip(rel, -MAX_REL, MAX_REL) + MAX_REL].astype(jnp.float32)
    scale = dh ** -0.5

    def one_chunk(args):
        c, qb = args
        start = c * CHUNK
        kb = lax.dynamic_slice_in_dim(kp, start, BAND, axis=1)
        vb = lax.dynamic_slice_in_dim(vp, start, BAND, axis=1)
        valid = (start - pad + jnp.arange(BAND)) >= 0
        sc = jnp.einsum('bqhd,bkhd->bhqk', qb, kb).astype(jnp.float32) * scale + bias
        sc = jnp.where(valid[None, None, None, :], sc, -jnp.inf)
        p = jax.nn.softmax(sc, axis=-1).astype(v.dtype)
        return jnp.einsum('bhqk,bkhd->bqhd', p, vb)

    out = lax.map(one_chunk, (jnp.arange(nc), qc))
    return jnp.moveaxis(out, 0, 1).reshape(b, s, h * dh)


def differential_attention(q, k, v, lam, norm_g, lambda_init):
    b, s, h, _, dh = q.shape
    nqb = s // Q_BLOCK
    qbs = jnp.moveaxis(q.reshape(b, nqb, Q_BLOCK, h, 2, dh), 1, 0)
    k_chunk = jnp.arange(s) // CHUNK
    scale = dh ** -0.5

    def one_block(args):
        n, qb = args
        q_chunk = (n * Q_BLOCK + jnp.arange(Q_BLOCK)) // CHUNK
        allowed = k_chunk[None, :] <= q_chunk[:, None]
        sc = jnp.einsum('bqhmd,bkhmd->bhmqk', qb, k).astype(jnp.float32) * scale
        sc = jnp.where(allowed, sc, -jnp.inf)
        p = jax.nn.softmax(sc, axis=-1)
        w = (p[:, :, 0] - lam * p[:, :, 1]).astype(v.dtype)
        return jnp.einsum('bhqk,bkhe->bqhe', w, v)

    o = lax.map(one_block, (jnp.arange(nqb), qbs))
    o = jnp.moveaxis(o, 0, 1).reshape(b, s, h, 2 * dh)
    o = rms_norm(o, norm_g) * (1.0 - lambda_init)
    return o.reshape(b, s, h * 2 * dh)


def memory_attention(q, k, v):
    b, s, h, dc = q.shape
    sc = jnp.einsum('bqhd,bkhd->bhqk', q, k).astype(jnp.float32) * dc ** -0.5
    p = jax.nn.softmax(sc, axis=-1).astype(v.dtype)
    return jnp.einsum('bhqk,bkhd->bqhd', p, v).reshape(b, s, h * dc)


def hybrid_mixer(x, mem, w_in, w_mem_kv, rel_bias, lambda_q1, lambda_k1, lambda_q2, lambda_k2,
                 diff_norm_g, w_branch_a, w_branch_b, w_branch_c, w_gates, b_gates, w_o,
                 lambda_init, cos, sin):
    b, s, d = x.shape
    proj = x @ w_in
    cuts = np.cumsum([A_WIDTH, A_WIDTH, A_WIDTH, B_QK_WIDTH, B_QK_WIDTH, B_V_WIDTH]).tolist()
    a_q, a_k, a_v, b_q, b_k, b_v, c_q = jnp.split(proj, cuts, axis=-1)

    shp_a = (b, s, A_HEADS, A_HEAD_DIM)
    y_a = chunk_relbias_attention(a_q.reshape(shp_a), a_k.reshape(shp_a), a_v.reshape(shp_a), rel_bias)

    shp_b = (b, s, B_HEADS, 2, B_HEAD_DIM)
    bq = apply_rope(b_q.reshape(shp_b), cos, sin)
    bk = apply_rope(b_k.reshape(shp_b), cos, sin)
    f32 = jnp.float32
    lam = (jnp.exp(jnp.sum(lambda_q1.astype(f32) * lambda_k1.astype(f32)))
           - jnp.exp(jnp.sum(lambda_q2.astype(f32) * lambda_k2.astype(f32))) + lambda_init)
    y_b = differential_attention(bq, bk, b_v.reshape(b, s, B_HEADS, 2 * B_HEAD_DIM), lam,
                                 diff_norm_g, lambda_init)

    m = mem.shape[1]
    c_k, c_v = jnp.split(mem @ w_mem_kv, 2, axis=-1)
    y_c = memory_attention(c_q.reshape(b, s, C_HEADS, C_HEAD_DIM),
                           c_k.reshape(b, m, C_HEADS, C_HEAD_DIM),
                           c_v.reshape(b, m, C_HEADS, C_HEAD_DIM))

    gates = jax.nn.sigmoid(x @ w_gates + b_gates).reshape(b, s, N_BRANCHES, d)
    merged = (gates[:, :, 0] * (y_a @ w_branch_a)
              + gates[:, :, 1] * (y_b @ w_branch_b)
              + gates[:, :, 2] * (y_c @ w_branch_c))
    return merged @ w_o


def moe_ffn(x, w_router, b_router, w_mlp1, b_mlp1, w_mlp2, b_mlp2):
    b, s, d = x.shape
    xt = x.reshape(-1, d)
    n = xt.shape[0]
    logits = (xt @ w_router + b_router).astype(jnp.float32)
    top_vals, top_idx = lax.top_k(logits, TOP_K)
    gates = jax.nn.softmax(top_vals, axis=-1).astype(x.dtype)

    flat_e = top_idx.reshape(-1)
    p = flat_e.shape[0]
    order = jnp.argsort(flat_e)
    sorted_e = flat_e[order]
    counts = jnp.bincount(flat_e, length=N_EXPERTS)
    padded = (counts + EXPERT_BLOCK - 1) // EXPERT_BLOCK * EXPERT_BLOCK
    start = jnp.cumsum(counts) - counts
    pend = jnp.cumsum(padded)
    pstart = pend - padded
    dest = pstart[sorted_e] + jnp.arange(p) - start[sorted_e]
    n_blocks = -(-p // EXPERT_BLOCK) + N_EXPERTS
    cap = n_blocks * EXPERT_BLOCK
    buf_tok = jnp.full((cap,), n, jnp.int32).at[dest].set((order // TOP_K).astype(jnp.int32))
    buf_gate = jnp.zeros((cap,), x.dtype).at[dest].set(gates.reshape(-1)[order])
    block_e = jnp.minimum(jnp.searchsorted(pend, jnp.arange(n_blocks) * EXPERT_BLOCK, side='right'),
                          N_EXPERTS - 1)
    x_pad = jnp.concatenate([xt, jnp.zeros((1, d), xt.dtype)], axis=0)

    def expert_block(args):
        tok, e = args
        xb = x_pad[tok]
        h = xb @ w_mlp1[e] + b_mlp1[e]
        x_glu = jnp.minimum(h[:, ::2], SWIGLU_LIMIT)
        x_lin = jnp.clip(h[:, 1::2], -SWIGLU_LIMIT, SWIGLU_LIMIT)
        act = x_glu * jax.nn.sigmoid(SWIGLU_ALPHA * x_glu) * (x_lin + 1.0)
        return act @ w_mlp2[e] + b_mlp2[e]

    y = lax.map(expert_block, (buf_tok.reshape(n_blocks, EXPERT_BLOCK), block_e))
    y = y.reshape(cap, d) * buf_gate[:, None]
    out = jnp.zeros((n + 1, d), x.dtype).at[buf_tok].add(y)[:n]
    return out.reshape(b, s, d)


def setup_inputs(seed: int = 0) -> dict:
    key = jax.random.key(seed)
    ks = jax.random.split(key, 32)
    f32 = jnp.float32
    L, D, E, F = DEPTH, D_MODEL, N_EXPERTS, D_EXPERT

    def nrm(k, shape, scale):
        return jax.random.normal(k, shape, f32) * scale

    return {
        'x': nrm(ks[0], (BATCH, SEQ, D), 1.0),
        'mem': nrm(ks[1], (BATCH, MEM_LEN, D), 1.0),
        'w_in': nrm(ks[2], (L, D, IN_WIDTH), D ** -0.5),
        'w_mem_kv': nrm(ks[3], (L, D, 2 * C_WIDTH), D ** -0.5),
        'rel_bias': nrm(ks[4], (L, A_HEADS, 2 * MAX_REL + 1), 0.5),
        'lambda_q1': nrm(ks[5], (L, B_HEAD_DIM), 0.1),
        'lambda_k1': nrm(ks[6], (L, B_HEAD_DIM), 0.1),
        'lambda_q2': nrm(ks[7], (L, B_HEAD_DIM), 0.1),
        'lambda_k2': nrm(ks[8], (L, B_HEAD_DIM), 0.1),
        'diff_norm_g': 1.0 + nrm(ks[9], (L, 2 * B_HEAD_DIM), 0.02),
        'w_branch_a': nrm(ks[10], (L, A_WIDTH, D), A_WIDTH ** -0.5 * DEEPNORM_BETA),
        'w_branch_b': nrm(ks[11], (L, B_V_WIDTH, D), B_V_WIDTH ** -0.5 * DEEPNORM_BETA),
        'w_branch_c': nrm(ks[12], (L, C_WIDTH, D), C_WIDTH ** -0.5 * DEEPNORM_BETA),
        'w_gates': nrm(ks[13], (L, D, N_BRANCHES * D), D ** -0.5),
        'b_gates': nrm(ks[14], (L, N_BRANCHES * D), 0.02),
        'w_o': nrm(ks[15], (L, D, D), D ** -0.5 * DEEPNORM_BETA),
        'ln1_g': 1.0 + nrm(ks[16], (L, D), 0.02),
        'ln1_b': nrm(ks[17], (L, D), 0.02),
        'w_router': nrm(ks[18], (L, D, E), D ** -0.5),
        'b_router': nrm(ks[19], (L, E), 0.01),
        'w_mlp1': nrm(ks[20], (L, E, D, 2 * F), D ** -0.5),
        'b_mlp1': nrm(ks[21], (L, E, 2 * F), 0.02),
        'w_mlp2': nrm(ks[22], (L, E, F, D), F ** -0.5 * DEEPNORM_BETA),
        'b_mlp2': nrm(ks[23], (L, E, D), 0.02),
        'ln2_g': 1.0 + nrm(ks[24], (L, D), 0.02),
        'ln2_b': nrm(ks[25], (L, D), 0.02),
    }


def reference(x, mem, w_in, w_mem_kv, rel_bias, lambda_q1, lambda_k1, lambda_q2, lambda_k2,
              diff_norm_g, w_branch_a, w_branch_b, w_branch_c, w_gates, b_gates, w_o,
              ln1_g, ln1_b, w_router, b_router, w_mlp1, b_mlp1, w_mlp2, b_mlp2, ln2_g, ln2_b):
    cos, sin = rope_tables(x.shape[1], B_HEAD_DIM, x.dtype)
    h = x
    for l in range(DEPTH):
        lambda_init = 0.8 - 0.6 * math.exp(-0.3 * l)
        mix = hybrid_mixer(h, mem, w_in[l], w_mem_kv[l], rel_bias[l], lambda_q1[l], lambda_k1[l],
                           lambda_q2[l], lambda_k2[l], diff_norm_g[l], w_branch_a[l], w_branch_b[l],
                           w_branch_c[l], w_gates[l], b_gates[l], w_o[l], lambda_init, cos, sin)
        h = layer_norm(DEEPNORM_ALPHA * h + mix, ln1_g[l], ln1_b[l])
        ffn = moe_ffn(h, w_router[l], b_router[l], w_mlp1[l], b_mlp1[l], w_mlp2[l], b_mlp2[l])
        h = layer_norm(DEEPNORM_ALPHA * h + ffn, ln2_g[l], ln2_b[l])
    return h
```

```python
import numpy as np
from contextlib import ExitStack
import concourse.bass as bass
import concourse.mybir as mybir
from concourse.bass_utils import run_bass_kernel_spmd

F32 = mybir.dt.float32
BF16 = mybir.dt.bfloat16
AF = mybir.ActivationFunctionType
ALU = mybir.AluOpType
AX = mybir.AxisListType

NCORE = 8
GSZ = 4
RG4 = [[0, 2, 4, 6], [1, 3, 5, 7]]
RG2 = [[0, 1], [2, 3], [4, 5], [6, 7]]
E_LOC = 16
S = 2048
D = 4096
KC = D // 128
MEM = 256
AH, BH, CH = 16, 4, 4
AHL, BHL, CHL = 8, 2, 2
E = 32
F = 1536
NEG = -30000.0
ALPHA = 2.0 ** 0.25
LAMBDA_INIT = 0.8 - 0.6
SCALE_A = 128.0 ** -0.5
SCALE_C = 256.0 ** -0.5
C_QA, C_KA, C_VA = 0, 2048, 4096
C_QB, C_QBS, C_KB, C_KBS, C_VB, C_QC = 6144, 7168, 8192, 9216, 10240, 11264
WIN = 12288
TB = 512
NTB = S // TB
R_QA, R_KA, R_QB, R_KB, R_QC = 0, 1024, 2048, 2560, 3072
SEM_LIMIT = 30000
NSPW = 8


class P:
    def __init__(self, nc, es):
        self.nc = nc
        self.es = es
        self.spw = ['spw%d' % i for i in range(NSPW)]
        self.spw_i = 0
        self.streams = ['pe', 'dve', 'act', 'sp', 'pool', 'cc', 'pre'] + self.spw
        self.lazy = {'pool', 'cc', 'pre'} | set(self.spw)
        self.eng = {'pe': nc.tensor, 'dve': nc.vector, 'act': nc.scalar, 'sp': nc.sync,
                    'pool': nc.gpsimd, 'cc': nc.gpsimd, 'pre': nc.gpsimd}
        self.en = {'pe': 'pe', 'dve': 'dve', 'act': 'act', 'sp': 'sp', 'pool': 'g', 'cc': 'g', 'pre': 'g'}
        self.unit = {'pe': 1, 'dve': 1, 'act': 1, 'sp': 16, 'pool': 16, 'cc': 1, 'pre': 16}
        for k in self.spw:
            self.eng[k] = nc.sync
            self.en[k] = 'sp'
            self.unit[k] = 16
        self.engs = {'pe': nc.tensor, 'dve': nc.vector, 'act': nc.scalar, 'sp': nc.sync, 'g': nc.gpsimd}
        self.nsem = 0
        self.sem = {k: self._newsem(k) for k in self.streams}
        self.cnt = {k: 0 for k in self.streams}
        self.epoch = {k: 0 for k in self.streams}
        self.seen = {e: {k: 0 for k in self.streams} for e in self.engs}
        self.n = 0

    def _newsem(self, k):
        self.nsem += 1
        return self.es.enter_context(self.nc.semaphore("sem_%s_%d" % (k, self.nsem)))

    def _roll(self, st):
        final = self.cnt[st] * self.unit[st]
        for e, eng in self.engs.items():
            eng.wait_ge(self.sem[st], final)
            self.seen[e][st] = 0
        self.sem[st] = self._newsem(st)
        self.cnt[st] = 0
        self.epoch[st] += 1

    def snap(self, streams=None):
        return {k: (self.epoch[k], self.cnt[k]) for k in (streams or self.streams)}

    def barrier_on(self, streams, snap=None):
        for k in streams:
            c = self.cnt[k]
            if snap is not None:
                ep, c = snap[k]
                if ep != self.epoch[k]:
                    continue
            for e, eng in self.engs.items():
                if c > self.seen[e][k]:
                    eng.wait_ge(self.sem[k], c * self.unit[k])
                    self.seen[e][k] = c

    def op(self, st, fn, nowait=(), after=()):
        if (self.cnt[st] + 1) * self.unit[st] > SEM_LIMIT:
            self._roll(st)
        e = self.en[st]
        eng = self.eng[st]
        need = {}
        for k in self.streams:
            if k in nowait:
                continue
            if k in self.lazy and st not in self.lazy:
                continue
            if k == st and st not in ('dve', 'act', 'cc'):
                continue
            need[k] = self.cnt[k]
        for sn in after:
            if sn is None:
                continue
            for k, (ep, c) in sn.items():
                if ep == self.epoch[k]:
                    need[k] = max(need.get(k, 0), c)
        for k, c in need.items():
            if c > self.seen[e][k]:
                eng.wait_ge(self.sem[k], c * self.unit[k])
                self.seen[e][k] = c
        ins = fn(eng)
        self.cnt[st] += 1
        self.n += 1
        if st == 'cc':
            ins.then_inc(self.sem[st])
        else:
            ins.then_inc(self.sem[st], self.unit[st])

    def mm(self, out, lhsT, rhs, start, stop, **kw):
        self.op('pe', lambda e: e.matmul(out, lhsT, rhs, start=start, stop=stop), **kw)

    def load(self, out, in_, **kw):
        self.op('sp', lambda e: e.dma_start(out=out, in_=in_), **kw)

    def loadw(self, out, in_, nowait=(), after=()):
        st = self.spw[self.spw_i % NSPW]
        self.spw_i += 1
        self.op(st, lambda e: e.dma_start(out=out, in_=in_), nowait=tuple(nowait) + tuple(self.spw), after=after)
        return self.snap([st])

    def loadc(self, out, in_, **kw):
        self.op('pool', lambda e: e.dma_start(out=out, in_=in_), **kw)

    def dve(self, fn, **kw):
        self.op('dve', fn, **kw)

    def act(self, fn, **kw):
        self.op('act', fn, **kw)

    def finish(self):
        e = self.nc.sync
        for k in self.streams:
            e.wait_ge(self.sem[k], self.cnt[k] * self.unit[k])


def _chunk_rows(r, c):
    rs = r // GSZ
    crow = max(1, (512 * 1024) // c)
    crow = min(crow, rs)
    while rs % crow:
        crow -= 1
    return rs, crow


def kc_view(ap, p=128):
    return ap.rearrange("(k p) c -> p k c", p=p)


def build(upto=99, dumps=()):
    nc = bass.Bass("TRN2", target_bir_lowering=False)
    es = ExitStack()
    p = P(nc, es)

    def din(name, shape):
        return nc.dram_tensor(name, list(shape), F32, kind="ExternalInput").ap()

    def dint(name, shape, dt=BF16):
        return nc.dram_tensor(name, list(shape), dt)

    uid = [0]

    def sb(st, name, shape, dt):
        uid[0] += 1
        return st.enter_context(nc.sbuf_tensor("%s_%d" % (name, uid[0]), list(shape), dt))

    def pbank(st, name, shape=(128, 512), dt=F32):
        uid[0] += 1
        return st.enter_context(nc.psum_tensor("%s_%d" % (name, uid[0]), list(shape), dt))

    xT = din("xT", [D, S])
    x = din("x", [S, D])
    memT = din("memT", [D, MEM])
    out = nc.dram_tensor("out", [S, D], F32, kind="ExternalOutput").ap()
    wshapes = {"w_in": (12 * 128, 16384), "w_memkv": (2 * 128, 16384), "w_mg": (KC * 128, 16384),
               "w_o": (8 * 128, 16384)}
    if upto >= 7:
        wshapes["w1"] = (E_LOC * 24 * 128, KC * 128)
        wshapes["w2"] = (E_LOC * 8 * 128, 12 * 512)
    wsh = {k: din(k + "_sh", [r // GSZ, c]) for k, (r, c) in wshapes.items()}
    biasA = din("biasA", [AHL, 5, 128, 128])
    cosT = din("cosT", [128, S])
    sinT = din("sinT", [128, S])
    lam4 = din("lam4", [4, 128])
    gB = din("gB", [128, 2])
    bg = din("bg", [128, 96])
    w_r = din("w_r", [D, E])
    b_r = din("b_r", [1, E])
    b1 = din("b1", [E_LOC, 128, 24])
    b2 = din("b2", [E_LOC, D])
    selA = din("selA", [128, 1])
    lng = din("lng", [4, D])
    ident_d = din("ident", [128, 128])

    qkT = dint("qkT", [3584, S]).ap()
    vA = dint("vA", [S, 1024]).ap()
    vB = dint("vB", [S, 512]).ap()
    kcT = dint("kcT", [512, MEM]).ap()
    vC = dint("vC", [MEM, 512]).ap()
    yT = dint("yT", [2048, S]).ap()
    hT = dint("hT", [D, S]).ap()
    hF = dint("hF", [S, D], F32).ap()
    gG = dint("gG", [S, E], F32).ap()
    scratch = {"qkT": qkT, "vA": vA, "vB": vB, "kcT": kcT, "vC": vC, "yT": yT, "hT": hT,
               "hF": hF, "gG": gG}
    dump_out = {}
    for nm in dumps:
        a = scratch[nm]
        dump_out[nm] = nc.dram_tensor("dump_" + nm, list(a.shape), a.dtype, kind="ExternalOutput").ap()

    xTb = dint("xTb", [D, S]).ap()
    memTb = dint("memTb", [D, MEM]).ap()
    b2b = dint("b2b", [E_LOC, D]).ap()
    for r0 in range(0, D, 512):
        p.op('pre', lambda e, r0=r0: e.dma_start(out=xTb[r0:r0 + 512, :], in_=xT[r0:r0 + 512, :]))
    p.op('pre', lambda e: e.dma_start(out=memTb, in_=memT))
    p.op('pre', lambda e: e.dma_start(out=b2b, in_=b2))

    wchunks = {}
    wcrow = {}
    wsnap = {}
    fz_in = dint("fence_in", [16, 128])
    fz_out = dint("fence_out", [16, 128])

    def fence():
        p.op('pre', lambda e: e.dma_start(out=fz_out.ap(), in_=fz_in.ap()))

    wbnc = {}

    def gather(k, i0=0, i1=None):
        r, c = wshapes[k]
        rs, crow = _chunk_rows(r, c)
        assert (GSZ * crow) % 128 == 0
        nch = rs // crow
        if k not in wbnc:
            wcrow[k] = GSZ * crow
            wchunks[k] = []
            wsnap[k] = []
            wbnc[k] = [dint("bnc_%s_%d" % (k, i), [crow, c]) for i in range(2)]
        bnc = wbnc[k]
        assert i0 == len(wchunks[k])
        for i in range(i0, nch if i1 is None else i1):
            gout = dint("gat_%s_%d" % (k, i), [GSZ * crow, c])
            wchunks[k].append(gout)
            p.loadc(bnc[i % 2].ap(), wsh[k][i * crow:(i + 1) * crow, :])
            p.op('cc', lambda e, a=bnc[i % 2], b=gout: e.collective_compute(
                "AllGather", ALU.bypass, replica_groups=RG4,
                ins=[a.ap().opt()], outs=[b.ap().opt()]))
            wsnap[k].append(p.snap(['cc']))

    def wready(k, t):
        return wsnap[k][(t * 128) // wcrow[k]]

    def wtile(k, t):
        g0 = t * 128
        return wchunks[k][g0 // wcrow[k]].ap()[g0 % wcrow[k]: g0 % wcrow[k] + 128, :]

    gather("w_in")
    fence()
    p.barrier_on(['pre', 'pool', 'cc'])
    for k in ("w_memkv", "w_mg", "w_o"):
        gather(k)
    fence()
    s_dense = p.snap(['pool', 'cc', 'pre'])
    def gather_experts(e0, e1):
        for ex in range(e0, e1):
            gather("w1", ex * 6, ex * 6 + 6)
            gather("w2", ex * 4, ex * 4 + 4)

    E_EARLY = 9
    if "w1" in wshapes:
        gather_experts(0, E_EARLY)

    ones = sb(es, "ones", [128, 128], BF16)
    p.dve(lambda e: e.memset(ones[:], 1.0))
    ident = sb(es, "identf", [128, 128], F32)
    p.load(ident[:], ident_d)

    SPW = tuple(p.spw)
    NOPE = ('dve', 'act', 'sp') + SPW
    NOEW = ('pe',) + SPW
    NOLD = ('pe', 'dve', 'act', 'sp', 'pool', 'cc')

    class Pipe:
        def __init__(self, wbufs, banks, s_scope):
            self.wbufs, self.banks, self.s_scope = wbufs, banks, s_scope
            self.ti = 0
            self.gi = 0
            self.pe_t = {}
            self.ld_t = {}
            self.ev_g = {}
            self.pending = None

        def _load(self, ti, src):
            buf = self.wbufs[ti % 2]
            rdy = None
            if isinstance(src, tuple):
                src, rdy = src
            self.ld_t[ti] = p.loadw(buf[:].rearrange("p k c -> p (k c)"), src, nowait=NOLD,
                                    after=[self.pe_t.get(ti - 2), self.s_scope, rdy])

        def run(self, tiles, extra=()):
            for n, (src, groups) in enumerate(tiles):
                ti = self.ti
                if ti not in self.ld_t:
                    self._load(ti, src)
                if n + 1 < len(tiles):
                    self._load(ti + 1, tiles[n + 1][0])
                buf = self.wbufs[ti % 2]
                for (n_ps, mm_fn, evac_fn) in groups:
                    gi = self.gi
                    ps = self.banks[(gi % 2) * 2:(gi % 2) * 2 + n_ps]
                    aft = [self.ld_t[ti], self.ev_g.get(gi - 2), self.s_scope] + list(extra)
                    first = True
                    for (o, l, r, st_, sp_) in mm_fn(buf, ps):
                        p.mm(o, l, r, st_, sp_, nowait=NOPE, after=aft if first else ())
                        first = False
                    pes = p.snap(['pe'])
                    evac_fn(ps, [pes, self.s_scope])
                    self.ev_g[gi] = p.snap(['dve', 'act'])
                    self.gi += 1
                self.pe_t[ti] = p.snap(['pe'])
                self.ti += 1

    def acc_mms(o, pairs):
        n = len(pairs)
        return [(o, l, r, i == 0, i == n - 1) for i, (l, r) in enumerate(pairs)]

    if upto >= 2:
        with ExitStack() as st:
            xb = sb(st, "xb", [128, KC, TB], BF16)
            wb = [sb(st, "wt", [128, KC, 512], BF16) for _ in range(2)]
            ev = sb(st, "ev", [128, 512], BF16)
            t1 = sb(st, "t1", [128, 512], F32)
            t2 = sb(st, "t2", [128, 512], F32)
            cs = sb(st, "cs", [128, TB], F32)
            sn = sb(st, "sn", [128, TB], F32)
            banks = [pbank(st, "ps2_") for _ in range(4)]
            pipe = Pipe(wb, banks, p.snap(['pe', 'dve', 'act', 'sp'] + list(SPW)))
            for tb in range(NTB):
                tsl = slice(tb * TB, (tb + 1) * TB)
                p.load(xb[:], kc_view(xTb[:, tsl]))
                p.load(cs[:], cosT[:, tsl])
                p.load(sn[:], sinT[:, tsl])
                s_in = p.snap(['sp'])
                tiles = []

                def fm_group(ci, r0):
                    def mm_fn(buf, ps):
                        return acc_mms(ps[0][:], [(buf[:, k, ci * 128:(ci + 1) * 128], xb[:, k, :]) for k in range(KC)])

                    def evac(ps, aft):
                        p.act(lambda e: e.copy(out=ev[:], in_=ps[0][:]), nowait=NOEW, after=aft)
                        p.load(qkT[r0:r0 + 128, tsl], ev[:])
                    return (1, mm_fn, evac)

                def rope_group(ci, r0):
                    def mm_fn(buf, ps):
                        return (acc_mms(ps[0][:], [(buf[:, k, ci * 128:(ci + 1) * 128], xb[:, k, :]) for k in range(KC)]) +
                                acc_mms(ps[1][:], [(buf[:, k, 256 + ci * 128:256 + (ci + 1) * 128], xb[:, k, :])
                                                   for k in range(KC)]))

                    def evac(ps, aft):
                        p.dve(lambda e: e.tensor_tensor(out=t1[:], in0=ps[0][:], in1=cs[:], op=ALU.mult),
                              nowait=NOEW, after=aft)
                        p.dve(lambda e: e.tensor_tensor(out=t2[:], in0=ps[1][:], in1=sn[:], op=ALU.mult), nowait=NOEW)
                        p.dve(lambda e: e.tensor_tensor(out=ev[:], in0=t1[:], in1=t2[:], op=ALU.add), nowait=NOEW)
                        p.load(qkT[r0:r0 + 128, tsl], ev[:])
                    return (2, mm_fn, evac)

                def tm_group(tt, dst, cb):
                    def mm_fn(buf, ps):
                        return acc_mms(ps[0][:], [(xb[:, k, tt * 128:(tt + 1) * 128], buf[:, k, :]) for k in range(KC)])

                    def evac(ps, aft):
                        p.act(lambda e: e.copy(out=ev[:], in_=ps[0][:]), nowait=NOEW, after=aft)
                        r0 = tb * TB + tt * 128
                        p.load(dst[r0:r0 + 128, cb:cb + 512], ev[:])
                    return (1, mm_fn, evac)

                def wt_(t):
                    return wtile("w_in", t)

                for i in range(2):
                    tiles.append((wt_(i), [fm_group(ci, R_QA + (i * 4 + ci) * 128) for ci in range(4)]))
                for i in range(2):
                    tiles.append((wt_(2 + i), [fm_group(ci, R_KA + (i * 4 + ci) * 128) for ci in range(4)]))
                tiles.append((wt_(4), [fm_group(ci, R_QC + ci * 128) for ci in range(4)]))
                for i in range(2):
                    tiles.append((wt_(5 + i), [rope_group(ci, R_QB + (i * 2 + ci) * 128) for ci in range(2)]))
                for i in range(2):
                    tiles.append((wt_(7 + i), [rope_group(ci, R_KB + (i * 2 + ci) * 128) for ci in range(2)]))
                for i in range(2):
                    tiles.append((wt_(9 + i), [tm_group(tt, vA, i * 512) for tt in range(4)]))
                tiles.append((wt_(11), [tm_group(tt, vB, 0) for tt in range(4)]))
                pipe.run(tiles, extra=[s_in])
            p.barrier_on(SPW)

    if upto >= 3:
        p.barrier_on(['pool', 'cc', 'pre'], snap=s_dense)
        with ExitStack() as st:
            mb = sb(st, "mb", [128, KC, MEM], BF16)
            wt = sb(st, "wt3", [128, KC, 512], BF16)
            ev = sb(st, "ev3", [128, 512], BF16)
            ps = pbank(st, "ps3")
            p.load(mb[:], kc_view(memTb))
            for c4 in range(0, 4, 4):
                p.load(wt[:].rearrange("p k c -> p (k c)"), wtile("w_memkv", 0))
                for ci in range(4):
                    for k in range(KC):
                        p.mm(ps[:, 0:MEM], wt[:, k, ci * 128:(ci + 1) * 128], mb[:, k, :], k == 0, k == KC - 1)
                    p.act(lambda e: e.copy(out=ev[:, 0:MEM], in_=ps[:, 0:MEM]))
                    r0 = (c4 + ci) * 128
                    p.load(kcT[r0:r0 + 128, :], ev[:, 0:MEM])
            for cb in range(0, 512, 512):
                p.load(wt[:].rearrange("p k c -> p (k c)"), wtile("w_memkv", 1))
                for tt in range(MEM // 128):
                    for k in range(KC):
                        p.mm(ps[:], mb[:, k, tt * 128:(tt + 1) * 128], wt[:, k, :], k == 0, k == KC - 1)
                    p.act(lambda e: e.copy(out=ev[:], in_=ps[:]))
                    p.load(vC[tt * 128:(tt + 1) * 128, cb:cb + 512], ev[:])

    if upto >= 4:
        with ExitStack() as st:
            qh = sb(st, "qh", [128, S], BF16)
            kh = sb(st, "kh", [128, S], BF16)
            vh = sb(st, "vh", [128, S // 128, 128], BF16)
            bt = sb(st, "bt", [128, 5, 128], F32)
            tmp = sb(st, "tmpa", [128, 128], F32)
            pT = sb(st, "pTa", [128, 5, 128], BF16)
            rec = sb(st, "reca", [128, 128], F32)
            yb = sb(st, "yba", [128, S], BF16)
            ps_s = pbank(st, "ps_sa")
            ps_o = pbank(st, "ps_oa")
            ps_d = pbank(st, "ps_da")
            for h in range(AHL):
                p.load(qh[:], qkT[R_QA + h * 128:R_QA + (h + 1) * 128, :])
                p.load(kh[:], qkT[R_KA + h * 128:R_KA + (h + 1) * 128, :])
                p.load(vh[:], vA[:, h * 128:(h + 1) * 128].rearrange("(t p) c -> p t c", p=128))
                p.load(bt[:], biasA[h].rearrange("w k q -> k w q"))
                for j in range(S // 128):
                    ws = [w for w in range(5) if j - 4 + w >= 0]
                    for w in ws:
                        kt = j - 4 + w
                        p.mm(ps_s[:, 0:128], kh[:, kt * 128:(kt + 1) * 128], qh[:, j * 128:(j + 1) * 128], True, True)
                        p.dve(lambda e, w=w: e.scalar_tensor_tensor(
                            out=tmp[:], in0=ps_s[:, 0:128], scalar=SCALE_A, in1=bt[:, w, :],
                            op0=ALU.mult, op1=ALU.add))
                        p.act(lambda e, w=w: e.activation(out=pT[:, w, :], in_=tmp[:], func=AF.Exp))
                    for i, w in enumerate(ws):
                        kt = j - 4 + w
                        p.mm(ps_o[:, 0:128], vh[:, kt, :], pT[:, w, :], i == 0, i == len(ws) - 1)
                    for i, w in enumerate(ws):
                        p.mm(ps_d[:, 0:128], ones[:], pT[:, w, :], i == 0, i == len(ws) - 1)
                    p.dve(lambda e: e.reciprocal(out=rec[:], in_=ps_d[:, 0:128]))
                    p.dve(lambda e, j=j: e.tensor_tensor(out=yb[:, j * 128:(j + 1) * 128], in0=ps_o[:, 0:128],
                                                         in1=rec[:], op=ALU.mult))
                p.load(yT[h * 128:(h + 1) * 128, :], yb[:])

        with ExitStack() as st:
            qh = sb(st, "qhb", [128, 2, S], BF16)
            kh = sb(st, "khb", [128, 2, S], BF16)
            vh = sb(st, "vhb", [128, S // 128, 256], BF16)
            pT = sb(st, "pTb", [128, S // 128, 128], BF16)
            rec = sb(st, "recb", [128, 128], F32)
            om = sb(st, "omb", [128, 2, 2, 128], F32)
            ob = sb(st, "ob", [128, 2, 128], F32)
            sq = sb(st, "sqb", [128, 2, 128], BF16)
            rs = sb(st, "rsb", [128, 128], F32)
            yb = sb(st, "ybb", [128, 2, S], BF16)
            l4 = sb(st, "l4", [128, 4, 128], F32)
            lt = sb(st, "lt", [128, 2, 128], F32)
            ls = sb(st, "ls", [128, 4], F32)
            gb = sb(st, "gbt", [128, 2], F32)
            ps_s = pbank(st, "ps_sb")
            ps_o0 = pbank(st, "ps_ob0")
            ps_o1 = pbank(st, "ps_ob1")
            ps_d = pbank(st, "ps_db")
            for i in range(4):
                p.load(l4[:, i, :], lam4[i:i + 1, :].partition_broadcast(128))
            p.dve(lambda e: e.tensor_tensor(out=lt[:, 0, :], in0=l4[:, 0, :], in1=l4[:, 1, :], op=ALU.mult))
            p.dve(lambda e: e.tensor_tensor(out=lt[:, 1, :], in0=l4[:, 2, :], in1=l4[:, 3, :], op=ALU.mult))
            p.dve(lambda e: e.reduce_sum(out=ls[:, 0:1], in_=lt[:, 0, :], axis=AX.X))
            p.dve(lambda e: e.reduce_sum(out=ls[:, 1:2], in_=lt[:, 1, :], axis=AX.X))
            p.act(lambda e: e.activation(out=ls[:, 2:4], in_=ls[:, 0:2], func=AF.Exp))
            p.dve(lambda e: e.tensor_tensor(out=ls[:, 0:1], in0=ls[:, 3:4], in1=ls[:, 2:3], op=ALU.subtract))
            p.dve(lambda e: e.tensor_scalar(out=ls[:, 0:1], in0=ls[:, 0:1], scalar1=-LAMBDA_INIT, scalar2=None,
                                            op0=ALU.add))
            p.load(gb[:], gB)
            p.dve(lambda e: e.tensor_scalar(out=gb[:], in0=gb[:], scalar1=(1.0 - LAMBDA_INIT), scalar2=None,
                                            op0=ALU.mult))
            for h in range(BHL):
                for m in range(2):
                    g = h * 2 + m
                    p.load(qh[:, m, :], qkT[R_QB + g * 128:R_QB + (g + 1) * 128, :])
                    p.load(kh[:, m, :], qkT[R_KB + g * 128:R_KB + (g + 1) * 128, :])
                p.load(vh[:], vB[:, h * 256:(h + 1) * 256].rearrange("(t p) c -> p t c", p=128))
                for j in range(S // 128):
                    for m in range(2):
                        for kt in range(j + 1):
                            p.mm(ps_s[:, 0:128], kh[:, m, kt * 128:(kt + 1) * 128],
                                 qh[:, m, j * 128:(j + 1) * 128], True, True)
                            p.act(lambda e, kt=kt: e.activation(out=pT[:, kt, :], in_=ps_s[:, 0:128],
                                                                func=AF.Exp, scale=SCALE_A))
                        p.dve(lambda e, j=j: e.memset(pT[64:128, j, 0:64], 0.0))
                        for kt in range(j + 1):
                            p.mm(ps_o0[:, 0:128], vh[:, kt, 0:128], pT[:, kt, :], kt == 0, kt == j)
                        for kt in range(j + 1):
                            p.mm(ps_o1[:, 0:128], vh[:, kt, 128:256], pT[:, kt, :], kt == 0, kt == j)
                        for kt in range(j + 1):
                            p.mm(ps_d[:, 0:128], ones[:], pT[:, kt, :], kt == 0, kt == j)
                        p.dve(lambda e: e.reciprocal(out=rec[:], in_=ps_d[:, 0:128]))
                        p.dve(lambda e, m=m: e.tensor_tensor(out=om[:, m, 0, :], in0=ps_o0[:, 0:128], in1=rec[:],
                                                             op=ALU.mult))
                        p.dve(lambda e, m=m: e.tensor_tensor(out=om[:, m, 1, :], in0=ps_o1[:, 0:128], in1=rec[:],
                                                             op=ALU.mult))
                    for c in range(2):
                        p.dve(lambda e, c=c: e.scalar_tensor_tensor(
                            out=ob[:, c, :], in0=om[:, 1, c, :], scalar=ls[:, 0:1], in1=om[:, 0, c, :],
                            op0=ALU.mult, op1=ALU.add))
                        p.dve(lambda e, c=c: e.tensor_tensor(out=sq[:, c, :], in0=ob[:, c, :], in1=ob[:, c, :],
                                                             op=ALU.mult))
                    p.mm(ps_d[:, 0:128], ones[:], sq[:, 0, :], True, False)
                    p.mm(ps_d[:, 0:128], ones[:], sq[:, 1, :], False, True)
                    p.dve(lambda e: e.tensor_scalar(out=rs[:], in0=ps_d[:, 0:128], scalar1=1.0 / 256.0, scalar2=1e-5,
                                                    op0=ALU.mult, op1=ALU.add))
                    p.act(lambda e: e.sqrt(out=rs[:], in_=rs[:]))
                    p.dve(lambda e: e.reciprocal(out=rs[:], in_=rs[:]))
                    for c in range(2):
                        p.dve(lambda e, c=c, j=j: e.scalar_tensor_tensor(
                            out=yb[:, c, j * 128:(j + 1) * 128], in0=ob[:, c, :], scalar=gb[:, c:c + 1], in1=rs[:],
                            op0=ALU.mult, op1=ALU.mult))
                for c in range(2):
                    r0 = 1024 + h * 256 + c * 128
                    p.load(yT[r0:r0 + 128, :], yb[:, c, :])

        with ExitStack() as st:
            qh = sb(st, "qhc", [128, 2, S], BF16)
            kh = sb(st, "khc", [128, 2, MEM], BF16)
            vh = sb(st, "vhc", [128, 2, 256], BF16)
            pT = sb(st, "pTc", [128, 2, 512], BF16)
            rec = sb(st, "recc", [128, 512], F32)
            yb = sb(st, "ybc", [128, 2, S], BF16)
            ps_s = pbank(st, "ps_sc")
            ps_o0 = pbank(st, "ps_oc0")
            ps_o1 = pbank(st, "ps_oc1")
            ps_d = pbank(st, "ps_dc")
            for h in range(CHL):
                for c in range(2):
                    r0 = h * 256 + c * 128
                    p.load(qh[:, c, :], qkT[R_QC + r0:R_QC + r0 + 128, :])
                    p.load(kh[:, c, :], kcT[r0:r0 + 128, :])
                p.load(vh[:], vC[:, h * 256:(h + 1) * 256].rearrange("(t p) c -> p t c", p=128))
                for qb in range(S // 512):
                    qs = slice(qb * 512, (qb + 1) * 512)
                    for kt in range(2):
                        for c in range(2):
                            p.mm(ps_s[:], kh[:, c, kt * 128:(kt + 1) * 128], qh[:, c, qs], c == 0, c == 1)
                        p.act(lambda e, kt=kt: e.activation(out=pT[:, kt, :], in_=ps_s[:], func=AF.Exp, scale=SCALE_C))
                    for kt in range(2):
                        p.mm(ps_o0[:], vh[:, kt, 0:128], pT[:, kt, :], kt == 0, kt == 1)
                    for kt in range(2):
                        p.mm(ps_o1[:], vh[:, kt, 128:256], pT[:, kt, :], kt == 0, kt == 1)
                    for kt in range(2):
                        p.mm(ps_d[:], ones[:], pT[:, kt, :], kt == 0, kt == 1)
                    p.dve(lambda e: e.reciprocal(out=rec[:], in_=ps_d[:]))
                    p.dve(lambda e, qs=qs: e.tensor_tensor(out=yb[:, 0, qs], in0=ps_o0[:], in1=rec[:], op=ALU.mult))
                    p.dve(lambda e, qs=qs: e.tensor_tensor(out=yb[:, 1, qs], in0=ps_o1[:], in1=rec[:], op=ALU.mult))
                for c in range(2):
                    r0 = 1536 + h * 256 + c * 128
                    p.load(yT[r0:r0 + 128, :], yb[:, c, :])

    yG = []
    s_yg = None
    if upto >= 4:
        for i in range(4):
            yin = dint("yin_%d" % i, [512, S])
            yg = dint("ygat_%d" % i, [1024, S])
            p.load(yin.ap(), yT[i * 512:(i + 1) * 512, :])
            p.op('cc', lambda e, a=yin, b=yg: e.collective_compute(
                "AllGather", ALU.bypass, replica_groups=RG2, ins=[a.ap().opt()], outs=[b.ap().opt()]))
            yG.append(yg)
        fence()
        s_yg = p.snap(['cc', 'pre'])
    if "w1" in wshapes:
        gather_experts(E_EARLY, E_LOC)
        fence()

    def layer_norm(st_tiles, src, g_t, b_t):
        s1, junk = st_tiles
        p.dve(lambda e: e.reduce_sum(out=s1[:, 0:1], in_=src, axis=AX.X))
        p.dve(lambda e: e.tensor_scalar(out=s1[:, 0:1], in0=s1[:, 0:1], scalar1=-1.0 / D, scalar2=None, op0=ALU.mult))
        p.dve(lambda e: e.tensor_scalar(out=src, in0=src, scalar1=s1[:, 0:1], scalar2=None, op0=ALU.add))
        p.dve(lambda e: e.tensor_tensor(out=junk[:], in0=src, in1=src, op=ALU.mult))
        p.dve(lambda e: e.reduce_sum(out=s1[:, 1:2], in_=junk[:], axis=AX.X))
        p.dve(lambda e: e.tensor_scalar(out=s1[:, 1:2], in0=s1[:, 1:2], scalar1=1.0 / D, scalar2=1e-5,
                                        op0=ALU.mult, op1=ALU.add))
        p.act(lambda e: e.sqrt(out=s1[:, 1:2], in_=s1[:, 1:2]))
        p.dve(lambda e: e.reciprocal(out=s1[:, 1:2], in_=s1[:, 1:2]))
        p.dve(lambda e: e.tensor_scalar(out=src, in0=src, scalar1=s1[:, 1:2], scalar2=None, op0=ALU.mult))
        p.dve(lambda e: e.tensor_tensor(out=src, in0=src, in1=g_t[:], op=ALU.mult))
        p.dve(lambda e: e.tensor_tensor(out=src, in0=src, in1=b_t[:], op=ALU.add))

    if upto >= 5:
        with ExitStack() as st0:
            mT = sb(st0, "mT", [128, KC, TB], BF16)
            bgt = sb(st0, "bgt", [128, 96], F32)
            p.load(bgt[:], bg)
            rng = [(0, 16), (16, 24), (24, 32)]
            for tb in range(NTB):
                tsl = slice(tb * TB, (tb + 1) * TB)
                with ExitStack() as st:
                    xb = sb(st, "xb5", [128, KC, TB], BF16)
                    yb = sb(st, "yb5", [128, KC, TB], BF16)
                    wb = [sb(st, "wmg", [128, KC, 512], BF16) for _ in range(2)]
                    gt = sb(st, "gt", [128, TB], F32)
                    macc = sb(st, "macc", [128, TB], F32)
                    tmp = sb(st, "tmp5", [128, TB], F32)
                    banks = [pbank(st, "ps5a") for _ in range(4)]
                    pipe = Pipe(wb, banks, p.snap(['pe', 'dve', 'act', 'sp'] + list(SPW)))
                    p.load(xb[:], kc_view(xTb[:, tsl]))
                    for i in range(4):
                        p.load(yb[:, i * 8:(i + 1) * 8, :], kc_view(yG[i].ap()[:, tsl]), after=[s_yg])
                    s_in = p.snap(['sp'])
                    tiles = []
                    for dc in range(KC):
                        groups = []
                        for i in range(3):
                            def mm_fn(buf, ps, i=i):
                                c0, c1 = rng[i]
                                return (acc_mms(ps[0][:], [(buf[:, k, 128 + i * 128:128 + (i + 1) * 128], xb[:, k, :])
                                                           for k in range(KC)]) +
                                        acc_mms(ps[1][:], [(buf[:, c, 0:128], yb[:, c, :]) for c in range(c0, c1)]))

                            def evac(ps, aft, i=i, dc=dc):
                                p.act(lambda e: e.activation(out=gt[:], in_=ps[0][:], func=AF.Sigmoid,
                                                             bias=bgt[:, i * 32 + dc:i * 32 + dc + 1]),
                                      nowait=NOEW, after=aft)
                                if i == 0:
                                    p.dve(lambda e: e.tensor_tensor(out=macc[:], in0=ps[1][:], in1=gt[:], op=ALU.mult),
                                          nowait=NOEW, after=aft)
                                else:
                                    p.dve(lambda e: e.tensor_tensor(out=tmp[:], in0=ps[1][:], in1=gt[:], op=ALU.mult),
                                          nowait=NOEW, after=aft)
                                    if i == 1:
                                        p.dve(lambda e: e.tensor_tensor(out=macc[:], in0=macc[:], in1=tmp[:], op=ALU.add),
                                              nowait=NOEW)
                                    else:
                                        p.dve(lambda e: e.tensor_tensor(out=mT[:, dc, :], in0=macc[:], in1=tmp[:],
                                                                        op=ALU.add), nowait=NOEW)
                            groups.append((2, mm_fn, evac))
                        tiles.append((wtile("w_mg", dc), groups))
                    pipe.run(tiles, extra=[s_in])
                    p.barrier_on(SPW)
                with ExitStack() as stB:
                    pre = sb(stB, "pre", [128, 4, D], F32)
                    with ExitStack() as st:
                        wb = [sb(st, "wo", [128, KC, 512], BF16) for _ in range(2)]
                        banks = [pbank(st, "ps5b") for _ in range(4)]
                        pipe = Pipe(wb, banks, p.snap(['pe', 'dve', 'act', 'sp'] + list(SPW)))
                        for t in range(4):
                            r0 = tb * TB + t * 128
                            p.load(pre[:, t, :], x[r0:r0 + 128, :])
                        s_in = p.snap(['sp'])
                        tiles = []
                        for cb in range(8):
                            groups = []
                            for t in range(4):
                                def mm_fn(buf, ps, t=t):
                                    return acc_mms(ps[0][:], [(mT[:, k, t * 128:(t + 1) * 128], buf[:, k, :]) for k in range(KC)])

                                def evac(ps, aft, t=t, cb=cb):
                                    p.dve(lambda e: e.scalar_tensor_tensor(
                                        out=pre[:, t, cb * 512:(cb + 1) * 512], in0=pre[:, t, cb * 512:(cb + 1) * 512],
                                        scalar=ALPHA, in1=ps[0][:], op0=ALU.mult, op1=ALU.add), nowait=NOEW, after=aft)
                                groups.append((1, mm_fn, evac))
                            tiles.append((wtile("w_o", cb), groups))
                        pipe.run(tiles, extra=[s_in])
                        p.barrier_on(SPW)
                    with ExitStack() as st:
                        lg = sb(st, "lg", [128, D], F32)
                        lb = sb(st, "lb", [128, D], F32)
                        junk = sb(st, "junk", [128, D], F32)
                        s1 = sb(st, "s1", [128, 2], F32)
                        hTt = sb(st, "hTt", [128, KC, 128], F32)
                        hTb = sb(st, "hTb", [128, KC, 128], BF16)
                        wr = sb(st, "wr", [128, KC, E], F32)
                        brt = sb(st, "brt", [128, E], F32)
                        lgt = sb(st, "lgt", [128, E], F32)
                        mx8 = sb(st, "mx8", [128, 8], F32)
                        msk = sb(st, "msk", [128, E], F32)
                        zz = sb(st, "zz", [128, E], F32)
                        sm = sb(st, "sm", [128, 2], F32)
                        ps_t = pbank(st, "ps_t")
                        ps_r = pbank(st, "ps_r")
                        p.load(lg[:], lng[0:1, :].partition_broadcast(128))
                        p.load(lb[:], lng[1:2, :].partition_broadcast(128))
                        p.load(wr[:], kc_view(w_r))
                        p.load(brt[:], b_r[0:1, :].partition_broadcast(128))
                        for t in range(4):
                            r0 = tb * TB + t * 128
                            layer_norm((s1, junk), pre[:, t, :], lg, lb)
                            p.load(hF[r0:r0 + 128, :], pre[:, t, :])
                            for k4 in range(0, KC, 4):
                                for kk in range(4):
                                    k = k4 + kk
                                    p.op('pe', lambda e, k=k, kk=kk, t=t: e.transpose(
                                        ps_t[:, kk * 128:(kk + 1) * 128], pre[:, t, k * 128:(k + 1) * 128], ident[:]))
                                p.act(lambda e, k4=k4: e.copy(out=hTt[:, k4:k4 + 4, :].rearrange("p k t -> p (k t)"),
                                                              in_=ps_t[:]))
                            p.dve(lambda e: e.tensor_copy(out=hTb[:], in_=hTt[:]))
                            p.load(kc_view(hT[:, r0:r0 + 128]), hTb[:])
                            for k in range(KC):
                                p.mm(ps_r[:, 0:E], hTt[:, k, :], wr[:, k, :], k == 0, k == KC - 1)
                            p.dve(lambda e: e.tensor_tensor(out=lgt[:], in0=ps_r[:, 0:E], in1=brt[:], op=ALU.add))
                            p.dve(lambda e: e.max(out=mx8[:], in_=lgt[:]))
                            p.dve(lambda e: e.tensor_scalar(out=msk[:], in0=lgt[:], scalar1=mx8[:, 3:4], scalar2=None,
                                                            op0=ALU.is_ge))
                            p.dve(lambda e: e.tensor_scalar(out=sm[:, 0:1], in0=mx8[:, 0:1], scalar1=-1.0, scalar2=None,
                                                            op0=ALU.mult))
                            p.act(lambda e: e.activation(out=zz[:], in_=lgt[:], func=AF.Exp, bias=sm[:, 0:1]))
                            p.dve(lambda e: e.tensor_tensor(out=zz[:], in0=zz[:], in1=msk[:], op=ALU.mult))
                            p.dve(lambda e: e.reduce_sum(out=sm[:, 1:2], in_=zz[:], axis=AX.X))
                            p.dve(lambda e: e.reciprocal(out=sm[:, 1:2], in_=sm[:, 1:2]))
                            p.dve(lambda e: e.tensor_scalar(out=zz[:], in0=zz[:], scalar1=sm[:, 1:2], scalar2=None,
                                                            op0=ALU.mult))
                            p.load(gG[r0:r0 + 128, :], zz[:])

    if upto >= 7:
        p.barrier_on(['pool', 'cc', 'pre'])
        with ExitStack() as st:
            hb = sb(st, "hb", [128, KC, TB], BF16)
            acc = sb(st, "acc", [128, 4, D], F32)
            Gt = sb(st, "Gt", [128, 4, E], F32)
            b1a = sb(st, "b1a", [128, E_LOC, 24], F32)
            p.load(b1a[:], b1.rearrange("e p j -> p e j"))
            selt = sb(st, "selt", [128, 1], F32)
            p.load(selt[:], selA)
            parts = []
            steps1 = [(tb, ex, j) for tb in range(NTB) for ex in range(E_LOC) for j in range(12)]
            steps2 = [(tb, ex, db) for tb in range(NTB) for ex in range(E_LOC) for db in range(8)]
            pe1 = {}
            ev1 = {}
            ld1 = {}
            pe2 = {}
            ld2 = {}
            pey = {}
            ev2 = {}
            ldb = {}
            pe2last = {}
            for tb in range(NTB):
                tsl = slice(tb * TB, (tb + 1) * TB)
                with ExitStack() as st2:
                    w1g = [sb(st2, "w1g", [128, KC, 128], BF16) for _ in range(2)]
                    w1l = [sb(st2, "w1l", [128, KC, 128], BF16) for _ in range(2)]
                    actT = [sb(st2, "actT", [128, 12, TB], BF16) for _ in range(2)]
                    w2t = [sb(st2, "w2t", [128, 12, 512], BF16) for _ in range(2)]
                    b2r = [sb(st2, "b2r", [1, D], BF16) for _ in range(2)]
                    tg = sb(st2, "tg", [128, TB], F32)
                    tsg = sb(st2, "tsg", [128, TB], F32)
                    tl = sb(st2, "tl", [128, TB], F32)
                    psg = [pbank(st2, "psg") for _ in range(2)]
                    psl = [pbank(st2, "psl") for _ in range(2)]
                    psy = [pbank(st2, "psy") for _ in range(2)]
                    s_tb = p.snap(['pe', 'dve', 'act', 'sp'] + list(SPW))
                    p.load(hb[:], kc_view(hT[:, tsl]))
                    for t in range(4):
                        r0 = tb * TB + t * 128
                        p.load(acc[:, t, :], hF[r0:r0 + 128, :])
                        p.load(Gt[:, t, :], gG[r0:r0 + 128, :])
                        p.dve(lambda e, t=t: e.tensor_scalar(out=acc[:, t, :], in0=acc[:, t, :], scalar1=selt[:, 0:1],
                                                             scalar2=None, op0=ALU.mult))
                    s_hb = p.snap(['sp'])

                    def load1(q):
                        _, ex, j = steps1[q]
                        aft = [pe1.get(q - 2), s_tb]
                        sa = p.loadw(w1g[q % 2][:].rearrange("p k c -> p (k c)"), wtile("w1", ex * 24 + j),
                                     nowait=NOLD, after=aft)
                        sb_ = p.loadw(w1l[q % 2][:].rearrange("p k c -> p (k c)"), wtile("w1", ex * 24 + 12 + j),
                                      nowait=NOLD, after=aft)
                        ld1[q] = {**sa, **sb_}

                    def load2(r):
                        _, ex, db = steps2[r]
                        ld2[r] = p.loadw(w2t[r % 2][:].rearrange("p j c -> p (j c)"), wtile("w2", ex * 8 + db),
                                         nowait=NOLD, after=[pe2.get(r - 2), s_tb])

                    q0 = tb * E_LOC * 12
                    r0g = tb * E_LOC * 8
                    load1(q0)
                    load2(r0g)
                    for ex in range(E_LOC):
                        ge = tb * E_LOC + ex
                        ldb[ge] = p.loadw(b2r[ge % 2][:], b2b[ex:ex + 1, :], nowait=NOLD,
                                          after=[pe2last.get(ge - 2), s_tb])
                        for j in range(12):
                            q = q0 + ex * 12 + j
                            if q + 1 < q0 + E_LOC * 12:
                                load1(q + 1)
                            g_, l_ = psg[q % 2], psl[q % 2]
                            aft = [ld1[q], ev1.get(q - 2), s_hb, s_tb]
                            for k in range(KC):
                                p.mm(g_[:], w1g[q % 2][:, k, :], hb[:, k, :], k == 0, k == KC - 1, nowait=NOPE,
                                     after=aft if k == 0 else ())
                            for k in range(KC):
                                p.mm(l_[:], w1l[q % 2][:, k, :], hb[:, k, :], k == 0, k == KC - 1, nowait=NOPE)
                            pe1[q] = p.snap(['pe'])
                            aw = [pe1[q], pe2last.get(ge - 2), s_tb]
                            p.dve(lambda e, j=j, ex=ex, g_=g_: e.tensor_scalar(
                                out=tg[:], in0=g_[:], scalar1=b1a[:, ex, j:j + 1], scalar2=7.0,
                                op0=ALU.add, op1=ALU.min), nowait=NOEW, after=aw)
                            p.act(lambda e: e.activation(out=tsg[:], in_=tg[:], func=AF.Sigmoid, scale=1.702),
                                  nowait=NOEW)
                            p.dve(lambda e, j=j, ex=ex, l_=l_: e.tensor_scalar(
                                out=tl[:], in0=l_[:], scalar1=b1a[:, ex, 12 + j:13 + j], scalar2=7.0,
                                op0=ALU.add, op1=ALU.min), nowait=NOEW)
                            p.dve(lambda e: e.tensor_scalar(out=tl[:], in0=tl[:], scalar1=-7.0, scalar2=1.0,
                                                            op0=ALU.max, op1=ALU.add), nowait=NOEW)
                            p.dve(lambda e: e.tensor_tensor(out=tg[:], in0=tg[:], in1=tsg[:], op=ALU.mult),
                                  nowait=NOEW)
                            p.dve(lambda e, j=j, ge=ge: e.tensor_tensor(out=actT[ge % 2][:, j, :], in0=tg[:],
                                                                        in1=tl[:], op=ALU.mult), nowait=NOEW)
                            ev1[q] = p.snap(['dve', 'act'])
                        act_ready = ev1[q0 + ex * 12 + 11]
                        for db in range(8):
                            r = r0g + ex * 8 + db
                            if r + 1 < r0g + E_LOC * 8:
                                load2(r + 1)
                            for t in range(4):
                                g = r * 4 + t
                                y_ = psy[g % 2]
                                aft = [ld2[r], ev2.get(g - 2), act_ready, ldb[ge], s_tb]
                                for j in range(12):
                                    p.mm(y_[:], actT[ge % 2][:, j, t * 128:(t + 1) * 128], w2t[r % 2][:, j, :],
                                         j == 0, False, nowait=NOPE, after=aft if j == 0 else ())
                                p.mm(y_[:], ones[0:1, :], b2r[ge % 2][0:1, db * 512:(db + 1) * 512], False, True,
                                     nowait=NOPE)
                                pey[g] = p.snap(['pe'])
                                p.dve(lambda e, t=t, db=db, ex=ex, y_=y_: e.scalar_tensor_tensor(
                                    out=acc[:, t, db * 512:(db + 1) * 512], in0=y_[:], scalar=Gt[:, t, ex:ex + 1],
                                    in1=acc[:, t, db * 512:(db + 1) * 512], op0=ALU.mult, op1=ALU.add),
                                    nowait=NOEW, after=[pey[g]])
                                ev2[g] = p.snap(['dve'])
                            pe2[r] = p.snap(['pe'])
                        pe2last[ge] = p.snap(['pe'])
                    p.barrier_on(SPW)
                for t in range(4):
                    pt = dint("part_%d_%d" % (tb, t), [128, D], F32)
                    pg = dint("pgat_%d_%d" % (tb, t), [256, D], F32)
                    p.load(pt.ap(), acc[:, t, :])
                    p.op('cc', lambda e, a=pt, b=pg: e.collective_compute(
                        "AllGather", ALU.bypass, replica_groups=RG2, ins=[a.ap().opt()], outs=[b.ap().opt()]))
                    parts.append((tb * TB + t * 128, pg, p.snap(['cc'])))
        fence()
        s_parts = p.snap(['cc', 'pre'])
        with ExitStack() as st2:
            lg = sb(st2, "lg7", [128, D], F32)
            lb = sb(st2, "lb7", [128, D], F32)
            junk = sb(st2, "junk7", [128, D], F32)
            a0 = sb(st2, "a0", [128, D], F32)
            a1 = sb(st2, "a1", [128, D], F32)
            s1 = sb(st2, "s17", [128, 2], F32)
            p.load(lg[:], lng[2:3, :].partition_broadcast(128))
            p.load(lb[:], lng[3:4, :].partition_broadcast(128))
            for (r0, pg, sn_) in parts:
                p.load(a0[:], pg.ap()[0:128, :], after=[sn_, s_parts])
                p.load(a1[:], pg.ap()[128:256, :], after=[sn_, s_parts])
                p.dve(lambda e: e.tensor_tensor(out=a0[:], in0=a0[:], in1=a1[:], op=ALU.add))
                layer_norm((s1, junk), a0[:], lg, lb)
                p.load(out[r0:r0 + 128, :], a0[:])
    else:
        with ExitStack() as st:
            xt = sb(st, "xt", [128, D], F32)
            p.load(xt[:], x[0:128, :])
            p.load(out[0:128, :], xt[:])

    for nm in dumps:
        a = scratch[nm]
        rows = a.shape[0]
        step = 512
        for r0 in range(0, rows, step):
            r1 = min(rows, r0 + step)
            p.op('pool', lambda e, r0=r0, r1=r1, nm=nm, a=a: e.dma_start(out=dump_out[nm][r0:r1, :], in_=a[r0:r1, :]))

    p.finish()
    es.close()
    return nc, p


def _interleave(w):
    r, c = w.shape
    rs, crow = _chunk_rows(r, c)
    nch = rs // crow
    v = w.reshape(nch, GSZ, crow, c)
    return [np.ascontiguousarray(v[:, r_].reshape(rs, c)) for r_ in range(GSZ)]


def prep(x, mem, w_in, w_mem_kv, rel_bias, lambda_q1, lambda_k1, lambda_q2, lambda_k2,
         diff_norm_g, w_branch_a, w_branch_b, w_branch_c, w_gates, b_gates, w_o,
         ln1_g, ln1_b, w_router, b_router, w_mlp1, b_mlp1, w_mlp2, b_mlp2, ln2_g, ln2_b, upto=99):
    f = np.float32
    x = np.asarray(x, f)
    wi = np.asarray(w_in[0], f)
    sizes = [2048, 2048, 2048, 1024, 1024, 1024, 1024]
    cuts = np.cumsum([0] + sizes)
    qa, ka, va, qb, kb, vb, qc = [wi[:, cuts[i]:cuts[i + 1]] for i in range(7)]

    def swap(w):
        v = w.reshape(D, 8, 2, 64)
        return np.ascontiguousarray(v[:, :, ::-1, :]).reshape(D, 1024)

    w_in_ext = np.concatenate([qa, ka, va, qb, swap(qb), kb, swap(kb), vb, qc], axis=1)

    def tile_cols(w, cols):
        sub = w[:, cols]
        kk = sub.shape[0] // 128
        return np.ascontiguousarray(sub.reshape(kk, 128, sub.shape[1]).transpose(1, 0, 2)).reshape(128, -1)

    ar = np.arange
    wkv = np.asarray(w_mem_kv[0], f)
    wsh_g = [dict(), dict()]
    for g in range(2):
        tl = []
        for base in (C_QA, C_KA):
            tl += [tile_cols(w_in_ext, ar(base + 1024 * g + i * 512, base + 1024 * g + (i + 1) * 512)) for i in range(2)]
        tl += [tile_cols(w_in_ext, ar(C_QC + 512 * g, C_QC + 512 * (g + 1)))]
        for base, bases in ((C_QB, C_QBS), (C_KB, C_KBS)):
            tl += [tile_cols(w_in_ext, np.concatenate([ar(base + 512 * g + i * 256, base + 512 * g + (i + 1) * 256),
                                                       ar(bases + 512 * g + i * 256, bases + 512 * g + (i + 1) * 256)]))
                   for i in range(2)]
        tl += [tile_cols(w_in_ext, ar(C_VA + 1024 * g + i * 512, C_VA + 1024 * g + (i + 1) * 512)) for i in range(2)]
        tl += [tile_cols(w_in_ext, ar(C_VB + 512 * g, C_VB + 512 * (g + 1)))]
        wsh_g[g]["w_in"] = _interleave(np.concatenate(tl, axis=0))
        del tl
        wsh_g[g]["w_memkv"] = _interleave(np.concatenate(
            [tile_cols(wkv, ar(512 * g, 512 * (g + 1))), tile_cols(wkv, ar(1024 + 512 * g, 1024 + 512 * (g + 1)))], axis=0))
    wfull = {}
    def orig_row(gg, r):
        if r < 1024:
            return 1024 * gg + r
        if r < 1536:
            return 2048 + 512 * gg + (r - 1024)
        return 3072 + 512 * gg + (r - 1536)
    yperm = np.array([orig_row((n % 1024) // 512, 512 * (n // 1024) + (n % 512)) for n in range(4096)])
    w_br = np.concatenate([np.asarray(w_branch_a[0], f), np.asarray(w_branch_b[0], f),
                           np.asarray(w_branch_c[0], f)], axis=0)[yperm]
    wg_ = np.asarray(w_gates[0], f)
    tl = []
    for dc in range(KC):
        tb_ = tile_cols(w_br, ar(dc * 128, (dc + 1) * 128)).reshape(128, KC, 128)
        tg_ = tile_cols(wg_, np.concatenate([ar(i * D + dc * 128, i * D + (dc + 1) * 128) for i in range(3)])
                        ).reshape(128, KC, 384)
        tl.append(np.concatenate([tb_, tg_], axis=2).reshape(128, KC * 512))
    wfull["w_mg"] = np.concatenate(tl, axis=0)
    del tl
    wo_ = np.asarray(w_o[0], f)
    wfull["w_o"] = np.concatenate([tile_cols(wo_, ar(cb * 512, (cb + 1) * 512)) for cb in range(8)], axis=0)
    wsh = {k: _interleave(v) for k, v in wfull.items()}
    del wfull
    if upto >= 7:
        w1 = np.asarray(w_mlp1[0], f)
        w2 = np.asarray(w_mlp2[0], f)
        for g in range(2):
            es = slice(g * E_LOC, (g + 1) * E_LOC)
            w1d = np.concatenate([w1[es, :, 0::2], w1[es, :, 1::2]], axis=2)
            t_ = np.ascontiguousarray(
                w1d.reshape(E_LOC, KC, 128, 24, 128).transpose(0, 3, 2, 1, 4)).reshape(E_LOC * 24 * 128, KC * 128)
            del w1d
            wsh_g[g]["w1"] = _interleave(t_)
            del t_
            t_ = np.ascontiguousarray(
                w2[es].reshape(E_LOC, 12, 128, 8, 512).transpose(0, 3, 2, 1, 4)).reshape(E_LOC * 8 * 128, 12 * 512)
            wsh_g[g]["w2"] = _interleave(t_)
            del t_
        del w1, w2
    kpos = np.arange(640)[:, None]
    qpos = np.arange(128)[None, :] + 512
    rel = np.clip(qpos - kpos, -128, 128) + 128
    rb = np.asarray(rel_bias[0], f)
    bias = rb[:, rel]
    kch = kpos // 64
    qch = qpos // 64
    ok = (kch <= qch) & (kch >= qch - 8)
    bias = np.where(ok[None], bias, np.float32(NEG)).astype(f).reshape(AH, 5, 128, 128)
    inv = 1.0 / (10000.0 ** (np.arange(0, 128, 2, dtype=np.float32) / 128))
    ang = np.arange(S, dtype=np.float32)[:, None] * inv[None, :]
    ang = np.concatenate([ang, ang], -1)
    cosT = np.ascontiguousarray(np.cos(ang).T.astype(f))
    sgn = np.concatenate([-np.ones(64, f), np.ones(64, f)])[:, None]
    sinT = np.ascontiguousarray((np.sin(ang).T * sgn).astype(f))
    lam4 = np.stack([np.asarray(v[0], f) for v in (lambda_q1, lambda_k1, lambda_q2, lambda_k2)])
    gBv = np.ascontiguousarray(np.asarray(diff_norm_g[0], f).reshape(2, 128).T)
    bgt = np.ascontiguousarray(np.asarray(b_gates[0], f).reshape(96, 128).T)
    b1v = np.asarray(b_mlp1[0], f)
    b1v = np.concatenate([b1v[:, 0::2], b1v[:, 1::2]], axis=1).reshape(E, 24, 128).transpose(0, 2, 1)
    b2v = np.asarray(b_mlp2[0], f)
    wr_ = np.asarray(w_router[0], f)
    br_ = np.asarray(b_router[0], f).reshape(1, E)
    lng = np.stack([np.asarray(v[0], f) for v in (ln1_g, ln1_b, ln2_g, ln2_b)])
    common = {"cosT": cosT, "sinT": sinT, "lam4": lam4, "gB": gBv, "bg": bgt,
              "lng": lng, "ident": np.eye(128, dtype=f)}
    grp = []
    for g in range(2):
        perm = np.concatenate([np.arange(g * E_LOC, (g + 1) * E_LOC), np.arange((1 - g) * E_LOC, (2 - g) * E_LOC)])
        grp.append({"w_r": np.ascontiguousarray(wr_[:, perm]), "b_r": np.ascontiguousarray(br_[:, perm]),
                    "b1": np.ascontiguousarray(b1v[g * E_LOC:(g + 1) * E_LOC]),
                    "b2": np.ascontiguousarray(b2v[g * E_LOC:(g + 1) * E_LOC]),
                    "biasA": np.ascontiguousarray(bias[g * AHL:(g + 1) * AHL]),
                    "selA": np.full((128, 1), ALPHA if g == 0 else 0.0, f)})
    in_maps = []
    for c in range(NCORE):
        bi, g = c // 2, c % 2
        m = dict(common)
        m.update(grp[g])
        m["xT"] = np.ascontiguousarray(x[bi].T)
        m["x"] = np.ascontiguousarray(x[bi])
        m["memT"] = np.ascontiguousarray(np.asarray(mem[bi], f).T)
        for k in wsh:
            m[k + "_sh"] = wsh[k][bi]
        for k in wsh_g[g]:
            m[k + "_sh"] = wsh_g[g][k][bi]
        in_maps.append(m)
    return in_maps


def kernel(**inputs):
    in_maps = prep(**inputs)
    nc, _ = build()
    res = run_bass_kernel_spmd(nc, in_maps, core_ids=list(range(NCORE)))
    return np.stack([np.asarray(res.results[2 * b]["out"], np.float32) for b in range(NCORE // 2)], axis=0)
```
